# Optimizing a Trainium2 kernel written in Bass

```python
import math
import jax, jax.numpy as jnp
from jax import lax
import numpy as np

D_MODEL = 1024
BATCH = 32
SEQ = 2048
DEPTH = 4

CTX_LEN = 256
GRID_W = 64
N_MIXERS = 3
DA_HEADS = 8
DA_HEAD_DIM = 64
DA_V_DIM = 2 * DA_HEAD_DIM
ROPE_F = DA_HEAD_DIM // 4
ROPE_THETA = 10000.0
Q_BLOCK = 128
FN_GROUPS = 8
FN_GROUP_DIM = D_MODEL // FN_GROUPS
HY_ORDER = 2
HY_EMB = 33
HY_BANDS = (HY_EMB - 1) // 2
HY_FILTER_HIDDEN = 64
HY_SIN_FREQ = 1.0
HY_FAST_DECAY = 0.3
HY_SLOW_DECAY = 1.5
HY_TARGET = 1e-2
HY_MAX_DECAY = math.log(HY_TARGET) / HY_FAST_DECAY
HY_MIN_DECAY = math.log(HY_TARGET) / HY_SLOW_DECAY
D_FF = 3584
N_EXPERTS = 8
TOP_K = 2
MOE_BLOCK = 128
EPS = 1e-6

kernel_name = 'hybrid_dit_diffattn_fnet_hyena_moe'


def rms_norm(x, g):
    xf = x.astype(jnp.float32)
    y = xf * lax.rsqrt(jnp.mean(xf * xf, axis=-1, keepdims=True) + EPS)
    return (y * g.astype(jnp.float32)).astype(x.dtype)


def axial_rope(x, cos, sin):
    xr = x.reshape(x.shape[:-1] + (2, 2, ROPE_F))
    cs = cos[None, :, None, None].astype(x.dtype)
    sn = sin[None, :, None, None].astype(x.dtype)
    x1, x2 = xr[..., 0, :], xr[..., 1, :]
    out = jnp.stack([x1 * cs - x2 * sn, x2 * cs + x1 * sn], axis=-2)
    return out.reshape(x.shape)


def diff_attention(h_l, h_c, cos, sin, w_qkv, lq1, lk1, lq2, lk2, subln_g, w_o, lam_init, with_ctx):
    B, L, D = h_l.shape

    def project(h):
        n = h.shape[1]
        q, k, v = jnp.split(h @ w_qkv, 3, axis=-1)
        return (q.reshape(B, n, DA_HEADS, 2, DA_HEAD_DIM),
                k.reshape(B, n, DA_HEADS, 2, DA_HEAD_DIM),
                v.reshape(B, n, DA_HEADS, DA_V_DIM))

    q_l, k_l, v_l = project(h_l)
    q_c, k_c, v_c = project(h_c)
    q_l = axial_rope(q_l, cos, sin)
    k_l = axial_rope(k_l, cos, sin)
    f32 = jnp.float32
    lam = (jnp.exp(jnp.sum(lq1.astype(f32) * lk1.astype(f32)))
           - jnp.exp(jnp.sum(lq2.astype(f32) * lk2.astype(f32))) + lam_init)
    scale = DA_HEAD_DIM ** -0.5

    def attend(q, k, v):
        n = q.shape[1]
        s = jnp.einsum('bqhnd,bkhnd->bnhqk', q, k).astype(f32) * scale
        p = jax.nn.softmax(s, axis=-1)
        a = (p[:, 0] - lam * p[:, 1]).astype(v.dtype)
        o = jnp.einsum('bhqk,bkhe->bqhe', a, v)
        o = rms_norm(o, subln_g) * (1.0 - lam_init)
        return o.reshape(B, n, DA_HEADS * DA_V_DIM)

    k_all = jnp.concatenate([k_c, k_l], axis=1)
    v_all = jnp.concatenate([v_c, v_l], axis=1)
    qb = q_l.reshape(B, L // Q_BLOCK, Q_BLOCK, DA_HEADS, 2, DA_HEAD_DIM).swapaxes(0, 1)
    o_l = lax.map(lambda q: attend(q, k_all, v_all), qb)
    y_l = o_l.swapaxes(0, 1).reshape(B, L, -1) @ w_o
    y_c = attend(q_c, k_c, v_c) @ w_o if with_ctx else None
    return y_l, y_c


def fourier_mix(h, w_o, b_o):
    B, L, D = h.shape
    hg = h.astype(jnp.float32).reshape(B, L, FN_GROUPS, FN_GROUP_DIM)
    f = jnp.fft.fftn(hg, axes=(1, 3), norm='ortho').real
    return f.reshape(B, L, D).astype(h.dtype) @ w_o + b_o


def short_conv(u, w, b):
    up = jnp.pad(u, ((0, 0), (1, 1), (0, 0)))
    return up[:, :-2] * w[0] + up[:, 1:-1] * w[1] + up[:, 2:] * w[2] + b


def hyena_filters(L, f_w1, f_b1, f_w2, f_b2, f_w3):
    f32 = jnp.float32
    D = f_w3.shape[-1] // (2 * HY_ORDER)
    t01 = jnp.linspace(0.0, 1.0, L, dtype=f32)[:, None]
    w = 2.0 * math.pi * jnp.arange(L, dtype=f32) / L
    bands = jnp.linspace(1e-4, HY_BANDS - 1, HY_BANDS, dtype=f32)
    fw = w[:, None] * bands[None, :]
    emb = jnp.concatenate([t01, jnp.cos(fw), -jnp.sin(fw)], axis=-1)
    hdn = jnp.sin(HY_SIN_FREQ * (emb @ f_w1.astype(f32) + f_b1.astype(f32)))
    hdn = jnp.sin(HY_SIN_FREQ * (hdn @ f_w2.astype(f32) + f_b2.astype(f32)))
    filt = (hdn @ f_w3.astype(f32)).reshape(L, HY_ORDER, 2, D)
    deltas = jnp.abs(jnp.linspace(HY_MIN_DECAY, HY_MAX_DECAY, D, dtype=f32))
    decay = jnp.exp(-t01 * deltas[None, :])
    filt = filt * decay[:, None, None, :]
    fwd, bwd = filt[:, :, 0], filt[:, :, 1]
    two = jnp.concatenate([fwd, jnp.zeros((1, HY_ORDER, D), f32), bwd[1:][::-1]], axis=0)
    return jnp.fft.rfft(two, axis=0)


def hyena_mixer(h, w_in, conv_w, conv_b, f_w1, f_b1, f_w2, f_b2, f_w3, f_bias, w_o):
    L = h.shape[1]
    u = short_conv(h @ w_in, conv_w, conv_b)
    x1, x2, v = jnp.split(u, 3, axis=-1)
    kf = hyena_filters(L, f_w1, f_b1, f_w2, f_b2, f_w3)
    z = v
    for n, gate in enumerate((x1, x2)):
        zf = jnp.fft.rfft(z.astype(jnp.float32), n=2 * L, axis=1)
        y = jnp.fft.irfft(zf * kf[None, :, n], n=2 * L, axis=1)[:, :L]
        z = gate * (y.astype(h.dtype) + f_bias[n] * z)
    return z @ w_o


def swiglu(h, w1, w3, w2):
    return (jax.nn.silu(h @ w1) * (h @ w3)) @ w2


def moe_swiglu(h, router, w1, w3, w2):
    T, D = h.shape
    logits = (h @ router).astype(jnp.float32)
    top_v, top_i = lax.top_k(logits, TOP_K)
    gates = jax.nn.softmax(top_v, axis=-1).astype(h.dtype)
    flat_e = top_i.reshape(-1)
    flat_w = gates.reshape(-1)
    flat_tok = jnp.repeat(jnp.arange(T, dtype=jnp.int32), TOP_K)
    TK = T * TOP_K
    counts = jnp.bincount(flat_e, length=N_EXPERTS)
    padded = (counts + MOE_BLOCK - 1) // MOE_BLOCK * MOE_BLOCK
    pad_start = jnp.cumsum(padded) - padded
    start = jnp.cumsum(counts) - counts
    order = jnp.argsort(flat_e)
    se = flat_e[order]
    dest = pad_start[se] + jnp.arange(TK, dtype=jnp.int32) - start[se]
    n_blocks = -(-TK // MOE_BLOCK) + N_EXPERTS
    n_rows = n_blocks * MOE_BLOCK
    row_tok = jnp.full((n_rows,), T, jnp.int32).at[dest].set(flat_tok[order])
    row_w = jnp.zeros((n_rows,), h.dtype).at[dest].set(flat_w[order])
    block_start = jnp.arange(n_blocks, dtype=jnp.int32) * MOE_BLOCK
    block_e = jnp.minimum(jnp.searchsorted(jnp.cumsum(padded), block_start, side='right'), N_EXPERTS - 1)
    h_ext = jnp.concatenate([h, jnp.zeros((1, D), h.dtype)], axis=0)
    xb = h_ext[row_tok].reshape(n_blocks, MOE_BLOCK, D)

    def expert_block(args):
        xe, e = args
        return swiglu(xe, w1[e], w3[e], w2[e])

    yb = lax.map(expert_block, (xb, block_e))
    y = jnp.zeros((T + 1, D), h.dtype).at[row_tok].add(yb.reshape(n_rows, D) * row_w[:, None])
    return y[:T]


def setup_inputs(seed: int = 0) -> dict:
    key = jax.random.key(seed)
    ks = iter(jax.random.split(key, 64))
    D = D_MODEL

    def nrm(shape, s):
        return jax.random.normal(next(ks), shape, jnp.float32) * s

    n_a = len(range(0, DEPTH, N_MIXERS))
    n_b = len(range(1, DEPTH, N_MIXERS))
    n_c = len(range(2, DEPTH, N_MIXERS))
    n_dense = len(range(0, DEPTH, 2))
    n_moe = len(range(1, DEPTH, 2))
    return {
        'x': nrm((BATCH, SEQ, D), 1.0),
        'c': nrm((BATCH, D), 1.0),
        'ctx': nrm((BATCH, CTX_LEN, D), 1.0),
        'c_ctx': nrm((D,), 1.0),
        'ada_w': nrm((DEPTH, D, 6 * D), 0.5 * D ** -0.5),
        'ada_b': nrm((DEPTH, 6 * D), 0.02),
        'norm1_g': 1.0 + nrm((DEPTH, D), 0.02),
        'norm2_g': 1.0 + nrm((DEPTH, D), 0.02),
        'attn_w_qkv': nrm((n_a, D, 3 * D), D ** -0.5),
        'attn_lq1': nrm((n_a, DA_HEAD_DIM), 0.1),
        'attn_lk1': nrm((n_a, DA_HEAD_DIM), 0.1),
        'attn_lq2': nrm((n_a, DA_HEAD_DIM), 0.1),
        'attn_lk2': nrm((n_a, DA_HEAD_DIM), 0.1),
        'attn_subln_g': 1.0 + nrm((n_a, DA_V_DIM), 0.02),
        'attn_w_o': nrm((n_a, D, D), D ** -0.5),
        'fnet_w_o': nrm((n_b, D, D), D ** -0.5),
        'fnet_b_o': nrm((n_b, D), 0.02),
        'hy_w_in': nrm((n_c, D, 3 * D), D ** -0.5),
        'hy_conv_w': nrm((n_c, 3, 3 * D), 0.5),
        'hy_conv_b': nrm((n_c, 3 * D), 0.02),
        'hy_f_w1': nrm((n_c, HY_EMB, HY_FILTER_HIDDEN), HY_EMB ** -0.5),
        'hy_f_b1': nrm((n_c, HY_FILTER_HIDDEN), 0.1),
        'hy_f_w2': nrm((n_c, HY_FILTER_HIDDEN, HY_FILTER_HIDDEN), HY_FILTER_HIDDEN ** -0.5),
        'hy_f_b2': nrm((n_c, HY_FILTER_HIDDEN), 0.1),
        'hy_f_w3': nrm((n_c, HY_FILTER_HIDDEN, HY_ORDER * 2 * D), 0.2 * HY_FILTER_HIDDEN ** -0.5),
        'hy_f_bias': nrm((n_c, HY_ORDER, D), 0.5),
        'hy_w_o': nrm((n_c, D, D), D ** -0.5),
        'ff_w1': nrm((n_dense, D, D_FF), D ** -0.5),
        'ff_w3': nrm((n_dense, D, D_FF), D ** -0.5),
        'ff_w2': nrm((n_dense, D_FF, D), D_FF ** -0.5),
        'moe_router': nrm((n_moe, D, N_EXPERTS), D ** -0.5),
        'moe_w1': nrm((n_moe, N_EXPERTS, D, D_FF), D ** -0.5),
        'moe_w3': nrm((n_moe, N_EXPERTS, D, D_FF), D ** -0.5),
        'moe_w2': nrm((n_moe, N_EXPERTS, D_FF, D), D_FF ** -0.5),
        'final_g': 1.0 + nrm((D,), 0.02),
    }


def reference(x, c, ctx, c_ctx, ada_w, ada_b, norm1_g, norm2_g,
              attn_w_qkv, attn_lq1, attn_lk1, attn_lq2, attn_lk2, attn_subln_g, attn_w_o,
              fnet_w_o, fnet_b_o,
              hy_w_in, hy_conv_w, hy_conv_b, hy_f_w1, hy_f_b1, hy_f_w2, hy_f_b2, hy_f_w3, hy_f_bias, hy_w_o,
              ff_w1, ff_w3, ff_w2, moe_router, moe_w1, moe_w3, moe_w2, final_g):
    B, L, D = x.shape
    f32 = jnp.float32
    grid_rows = L // GRID_W
    row = jnp.repeat(jnp.arange(grid_rows, dtype=f32), GRID_W)
    col = jnp.tile(jnp.arange(GRID_W, dtype=f32), grid_rows)
    freqs = ROPE_THETA ** (-jnp.arange(ROPE_F, dtype=f32) / ROPE_F)
    ang = jnp.stack([row[:, None] * freqs, col[:, None] * freqs], axis=1)
    cos, sin = jnp.cos(ang), jnp.sin(ang)
    silu_c = jax.nn.silu(c)
    silu_cc = jax.nn.silu(c_ctx)

    for i in range(DEPTH):
        last = i == DEPTH - 1
        kind, j = i % N_MIXERS, i // N_MIXERS
        mod_l = (silu_c @ ada_w[i] + ada_b[i])[:, None, :]
        mod_c = silu_cc @ ada_w[i] + ada_b[i]
        sh1_l, sc1_l, g1_l, sh2_l, sc2_l, g2_l = jnp.split(mod_l, 6, axis=-1)
        sh1_c, sc1_c, g1_c, sh2_c, sc2_c, g2_c = jnp.split(mod_c, 6, axis=-1)

        h_l = rms_norm(x, norm1_g[i]) * (1.0 + sc1_l) + sh1_l
        h_c = rms_norm(ctx, norm1_g[i]) * (1.0 + sc1_c) + sh1_c
        if kind == 0:
            y_l, y_c = diff_attention(h_l, h_c, cos, sin, attn_w_qkv[j], attn_lq1[j], attn_lk1[j],
                                      attn_lq2[j], attn_lk2[j], attn_subln_g[j], attn_w_o[j],
                                      0.8 - 0.6 * math.exp(-0.3 * i), not last)
        elif kind == 1:
            y_l = fourier_mix(h_l, fnet_w_o[j], fnet_b_o[j])
            y_c = None if last else fourier_mix(h_c, fnet_w_o[j], fnet_b_o[j])
        else:
            hp = (hy_w_in[j], hy_conv_w[j], hy_conv_b[j], hy_f_w1[j], hy_f_b1[j], hy_f_w2[j],
                  hy_f_b2[j], hy_f_w3[j], hy_f_bias[j], hy_w_o[j])
            y_l = hyena_mixer(h_l, *hp)
            y_c = None if last else hyena_mixer(h_c, *hp)
        x = x + g1_l * y_l
        if not last:
            ctx = ctx + g1_c * y_c

        h2_l = (rms_norm(x, norm2_g[i]) * (1.0 + sc2_l) + sh2_l).reshape(B * L, D)
        if last:
            tokens = h2_l
        else:
            h2_c = (rms_norm(ctx, norm2_g[i]) * (1.0 + sc2_c) + sh2_c).reshape(-1, D)
            tokens = jnp.concatenate([h2_l, h2_c], axis=0)
        if i % 2 == 0:
            k = i // 2
            f = swiglu(tokens, ff_w1[k], ff_w3[k], ff_w2[k])
        else:
            k = i // 2
            f = moe_swiglu(tokens, moe_router[k], moe_w1[k], moe_w3[k], moe_w2[k])
        x = x + g2_l * f[:B * L].reshape(B, L, D)
        if not last:
            ctx = ctx + g2_c * f[B * L:].reshape(B, -1, D)

    return rms_norm(x, final_g)
```

```python
from concourse.bass_utils import run_bass_kernel_spmd
import bisect
import numpy as np
import concourse.bass as bass
import concourse.mybir as mybir

F32 = mybir.dt.float32
BF16 = mybir.dt.bfloat16
AF = mybir.ActivationFunctionType
ALU = mybir.AluOpType
AX = mybir.AxisListType


class Res:
    __slots__ = ("name", "w", "r", "psum", "wd", "rd")

    def __init__(self, name="", psum=False):
        self.name = name
        self.psum = psum
        self.w = None
        self.r = {}
        self.wd = []
        self.rd = []


class Eng:
    def __init__(self, kb, name, h, sem, is_pe=False):
        self.kb = kb
        self.name = name
        self.h = h
        self.sem = sem
        self.is_pe = is_pe
        self.n = 0
        self.sig = []
        self.waited = {}

    def event_for(self, idx):
        p = bisect.bisect_left(self.sig, idx)
        assert p < len(self.sig), f"no signalled instr >= {idx} on {self.name}"
        return (self.sem, p + 1)


class DmaQ:
    def __init__(self, kb, name, issuer, sems):
        self.kb = kb
        self.name = name
        self.issuer = issuer
        self.sems = sems
        self.j = 0
        self.events = []
        self.is_pe = False

    def event_for(self, idx):
        return self.events[idx]


class KB:
    def __init__(self):
        self.nc = bass.Bass("TRN2", target_bir_lowering=False)
        nc = self.nc
        self._stack = []
        import contextlib
        self.es = contextlib.ExitStack()
        mk = lambda n: self.es.enter_context(nc.semaphore(n))
        self.pe = Eng(self, "pe", nc.tensor, mk("s_pe"), is_pe=True)
        self.act = Eng(self, "act", nc.scalar, mk("s_act"))
        self.dve = Eng(self, "dve", nc.vector, mk("s_dve"))
        self.pool = Eng(self, "pool", nc.gpsimd, mk("s_pool"))
        self.sp = Eng(self, "sp", nc.sync, mk("s_sp"))
        R = 8
        self.q_sp = DmaQ(self, "q_sp", self.sp, [mk(f"s_qsp{i}") for i in range(R)])
        self.q_pool = DmaQ(self, "q_pool", self.pool, [mk(f"s_qpl{i}") for i in range(R)])
        self.q_act = DmaQ(self, "q_act", self.act, [mk(f"s_qac{i}") for i in range(R)])
        self.engs = [self.pe, self.act, self.dve, self.pool, self.sp]
        self.qs = [self.q_sp, self.q_pool, self.q_act]
        self.n_wait = 0

    def _wait(self, eng, sem, val):
        k = id(sem)
        if eng.waited.get(k, 0) >= val:
            return
        eng.h.wait_ge(sem, val)
        eng.waited[k] = val
        self.n_wait += 1

    def _deps(self, eng, reads, writes):
        deps = {}
        ddeps = set()

        def add(e, i):
            if isinstance(e, DmaQ):
                ddeps.add((e, i))
            else:
                deps[e] = max(deps.get(e, -1), i)
        is_dma = getattr(self, "_cur_is_dma", False)
        for r in reads:
            if r.w is not None:
                add(*r.w)
            for e, i in r.wd:
                add(e, i)
            if r.psum:
                for e, i in r.r.items():
                    if e is not eng:
                        add(e, i)
        for w in writes:
            if w.w is not None:
                add(*w.w)
            if not is_dma:
                for e, i in w.wd:
                    add(e, i)
            for e, i in w.r.items():
                add(e, i)
            for e, i in w.rd:
                add(e, i)
        for e, i in deps.items():
            if e is eng and eng.is_pe:
                continue
            sem, val = e.event_for(i)
            self._wait(eng, sem, val)
        for e, i in ddeps:
            sem, val = e.events[i]
            self._wait(eng, sem, val)

    def _mark(self, src, idx, reads, writes):
        dma = isinstance(src, DmaQ)
        for r in reads:
            if dma:
                r.rd.append((src, idx))
            else:
                r.r[src] = idx
        for w in writes:
            if dma:
                if w.r or w.rd or w.w is not None:
                    w.wd = []
                w.wd.append((src, idx))
                w.w = None
            else:
                w.w = (src, idx)
                w.wd = []
            w.r = {}
            w.rd = []

    def op(self, eng, fn, reads=(), writes=(), signal=True):
        self._deps(eng, reads, writes)
        ins = fn(eng.h)
        idx = eng.n
        eng.n += 1
        if signal:
            ins.then_inc(eng.sem, 1)
            eng.sig.append(idx)
        self._mark(eng, idx, reads, writes)
        return ins

    def dma(self, q, out, in_, reads=(), writes=(), **kw):
        eng = q.issuer
        self._cur_is_dma = True
        self._deps(eng, reads, writes)
        self._cur_is_dma = False
        R = len(q.sems)
        j = q.j
        sem = q.sems[j % R]
        if j >= R:
            self._wait(eng, sem, 16 * (j // R))
        ins = eng.h.dma_start(out=out, in_=in_, **kw)
        ins.then_inc(sem, 16)
        q.events.append((sem, 16 * (j // R + 1)))
        q.j += 1
        eng.n += 0
        self._mark(q, j, reads, writes)
        return ins

    def barrier(self, engs=None):
        targets = []
        for e in self.engs:
            if e.sig:
                targets.append((e.sem, len(e.sig)))
        for q in self.qs:
            R = len(q.sems)
            for jj in range(max(0, q.j - R), q.j):
                targets.append(q.events[jj])
        for e in (engs or self.engs):
            for sem, val in targets:
                if sem is e.sem:
                    if e.is_pe:
                        continue
                self._wait(e, sem, val)

    def mm(self, out, lhsT, rhs, start, stop, reads=(), writes=(), signal=None):
        if signal is None:
            signal = stop
        return self.op(self.pe, lambda h: h.matmul(out, lhsT, rhs, start=start, stop=stop),
                       reads, writes, signal)

    def transpose(self, out, in_, ident, reads=(), writes=(), signal=True):
        return self.op(self.pe, lambda h: h.transpose(out, in_, ident), reads, writes, signal)

    def activation(self, out, in_, func, reads=(), writes=(), bias=None, scale=None, accum_out=None, eng=None):
        kw = {}
        if bias is not None:
            kw["bias"] = bias
        if scale is not None:
            kw["scale"] = scale
        if accum_out is not None:
            kw["accum_out"] = accum_out
        return self.op(eng or self.act, lambda h: h.activation(out=out, in_=in_, func=func, **kw), reads, writes)

    def tt(self, eng, out, in0, in1, op, reads=(), writes=()):
        return self.op(eng, lambda h: h.tensor_tensor(out=out, in0=in0, in1=in1, op=op), reads, writes)

    def ts(self, eng, out, in0, s1, s2, op0, op1=None, reads=(), writes=(), accum_out=None):
        kw = {}
        if op1 is not None:
            kw["op1"] = op1
        if accum_out is not None:
            kw["accum_out"] = accum_out
        return self.op(eng, lambda h: h.tensor_scalar(out=out, in0=in0, scalar1=s1, scalar2=s2, op0=op0, **kw),
                       reads, writes)

    def stt(self, eng, out, in0, scalar, in1, op0, op1, reads=(), writes=()):
        return self.op(eng, lambda h: h.scalar_tensor_tensor(out=out, in0=in0, scalar=scalar, in1=in1,
                                                             op0=op0, op1=op1), reads, writes)

    def copy(self, eng, out, in_, reads=(), writes=()):
        if eng is self.act:
            return self.op(eng, lambda h: h.copy(out=out, in_=in_), reads, writes)
        return self.op(eng, lambda h: h.tensor_copy(out=out, in_=in_), reads, writes)

    def memset(self, eng, ap, val, writes=()):
        return self.op(eng, lambda h: h.memset(ap, val), (), writes)


class Buf:
    _uid = [0]

    def __init__(self, kb, stack, name, shape, dtype, psum=False):
        nc = kb.nc
        Buf._uid[0] += 1
        name = f"{name}_{Buf._uid[0]}"
        if psum:
            self.t = stack.enter_context(nc.psum_tensor(name, shape, dtype))
        else:
            self.t = stack.enter_context(nc.sbuf_tensor(name, shape, dtype))
        self.res = Res(name, psum=psum)
        self.shape = shape

    def __getitem__(self, k):
        return self.t[k]


import math
import contextlib
import numpy as np
import ml_dtypes

D = 1024
DC = 8
DFF = 3584
FC = 28
NE = 8
EPS = 1e-6
NPBF = ml_dtypes.bfloat16


class Seg:
    def __init__(self, mcol, col0, n, b, is_ctx, pos0):
        self.mcol, self.col0, self.n, self.b, self.is_ctx, self.pos0 = mcol, col0, n, b, is_ctx, pos0


def make_consts(L, CTX):
    c = {}
    c["ones_bf"] = np.ones((128, 128), NPBF)
    c["ones_f"] = np.ones((128, 128), np.float32)
    c["ident_f"] = np.eye(128, dtype=np.float32)
    c["ident_bf"] = np.eye(128, dtype=np.float32).astype(NPBF)
    sel = np.zeros((8, 8, 128), np.float32)
    for e in range(8):
        sel[e, e, :] = 1.0
    c["sel"] = sel
    t = np.arange(L)
    row = (t // 64).astype(np.float32)
    col = (t % 64).astype(np.float32)
    freqs = (10000.0 ** (-np.arange(16, dtype=np.float32) / 16)).astype(np.float32)
    ang = np.stack([row[:, None] * freqs, col[:, None] * freqs], axis=1).astype(np.float32)
    cos, sin = np.cos(ang), np.sin(ang)
    COS = np.zeros((64, L), np.float32)
    SIN = np.zeros((64, L), np.float32)
    for axis in range(2):
        for half in range(2):
            for f in range(16):
                d = axis * 32 + half * 16 + f
                COS[d] = cos[:, axis, f]
                SIN[d] = (-sin[:, axis, f]) if half == 0 else sin[:, axis, f]
    c["rope_cos"] = np.concatenate([COS, COS], 0).astype(NPBF)
    c["rope_sin"] = np.concatenate([SIN, SIN], 0).astype(NPBF)
    P = np.zeros((128, 128), np.float32)
    for m in range(128):
        P[m ^ 16, m] = 1.0
    c["rope_perm"] = P.astype(NPBF)
    return c


class Builder:
    def __init__(self, NB, L, CTX, DEPTH):
        self.NB, self.L, self.CTX, self.DEPTH = NB, L, CTX, DEPTH
        self.NM = NB + 1
        self.TOK = NB * (L + CTX)
        self.kb = KB()
        kb = self.kb
        nc = kb.nc
        self.d = {}
        self.consts = make_consts(L, CTX)
        if DEPTH > 1:
            self.consts.update(fnet_consts(L, CTX))
        if DEPTH > 2:
            self.consts.update(hyena_consts(L, CTX))
        n_a = len(range(0, DEPTH, 3))
        n_b = len(range(1, DEPTH, 3))
        n_c = len(range(2, DEPTH, 3))
        n_dense = len(range(0, DEPTH, 2))
        n_moe = len(range(1, DEPTH, 2))

        def din(name, shape, dt=F32):
            self.d[name] = nc.dram_tensor(name, list(shape), dt, kind="ExternalInput").ap()
        din("xs_in", [D, self.TOK])
        din("cT", [128, 8, self.NM])
        din("ada_w", [DEPTH, D, 6 * D])
        din("ada_bT", [DEPTH, 128, 48])
        din("n1gT", [DEPTH, 128, 8])
        din("n2gT", [DEPTH, 128, 8])
        din("fgT", [128, 8])
        if n_a:
            din("attn_w_qkv", [n_a, D, 3 * D])
            din("attn_lqk", [n_a, 64, 4])
            din("attn_sublnT", [n_a, 128, 1])
            din("attn_w_o", [n_a, D, D])
        if n_b:
            din("fnet_w_o", [n_b, D, D])
            din("fnet_boT", [n_b, 128, 8])
        if n_c:
            din("hy_w_in", [n_c, D, 3 * D])
            din("hy_conv_w", [n_c, 3, 3 * D])
            din("hy_conv_b", [n_c, 3 * D])
            din("hy_f_w1", [n_c, 33, 64])
            din("hy_f_w2", [n_c, 64, 64])
            din("hy_f_w3", [n_c, 64, 4096])
            din("hy_f_b12T", [n_c, 64, 2])
            din("hy_f_bias", [n_c, 2, D])
            din("hy_w_o", [n_c, D, D])
        if n_dense:
            din("ff_w1", [n_dense, D, DFF])
            din("ff_w3", [n_dense, D, DFF])
            din("ff_w2", [n_dense, DFF, D])
        if n_moe:
            din("moe_router", [n_moe, D, NE])
            din("moe_w1", [n_moe, NE, D, DFF])
            din("moe_w3", [n_moe, NE, D, DFF])
            din("moe_w2", [n_moe, NE, DFF, D])
        for k, v in self.consts.items():
            din("c_" + k, v.shape, BF16 if v.dtype == NPBF else F32)
        self.out = nc.dram_tensor("out_T", [D, NB * L], F32, kind="ExternalOutput").ap()
        self.xs = nc.dram_tensor("xs", [D, self.TOK], F32).ap()
        self.qT_d = nc.dram_tensor("qT_d", [D, L + CTX], BF16).ap()
        self.xres = [Res(f"x{j}") for j in range((self.TOK + 127) // 128)]
        self.ores = [Res(f"o{j}") for j in range((NB * L + 127) // 128)]
        self.qres = Res("qT_d")
        self.segs = []
        for b in range(NB):
            for t0 in range(0, L, 512):
                self.segs.append(Seg(b, b * L + t0, min(512, L - t0), b, False, t0))
        for b in range(NB):
            self.segs.append(Seg(NB, NB * L + b * CTX, CTX, b, True, 0))

        self.gs = contextlib.ExitStack()
        st = self.gs
        T = lambda n, s, dt, psum=False: Buf(kb, st, n, s, dt, psum)
        self.ps = [T(f"ps{i}", [128, 512], F32, True) for i in range(8)]
        self.ones_bf = T("ones_bf", [128, 128], BF16)
        self.ones_f = T("ones_f", [128, 128], F32)
        self.ident_f = T("ident_f", [128, 128], F32)
        self.eps = T("eps", [128, 1], F32)
        self.zero = T("zero", [128, 1], F32)
        self.silu_c = T("silu_c", [128, 8, self.NM], F32)
        self.mod = T("mod", [128, 48, self.NM], F32)
        self.G1 = T("G1", [128, 8, self.NM], F32)
        self.G2 = T("G2", [128, 8, self.NM], F32)
        self.fg = T("fg", [128, 8, 1], F32)
        q = kb.q_sp
        kb.dma(q, self.ones_bf[:], self.d["c_ones_bf"][:, :], writes=[self.ones_bf.res])
        kb.dma(q, self.ones_f[:], self.d["c_ones_f"][:, :], writes=[self.ones_f.res])
        kb.dma(q, self.ident_f[:], self.d["c_ident_f"][:, :], writes=[self.ident_f.res])
        kb.dma(q, self.fg[:, :, 0], self.d["fgT"][:, :], writes=[self.fg.res])
        kb.memset(kb.dve, self.eps[:], EPS, writes=[self.eps.res])
        kb.memset(kb.dve, self.zero[:], 0.0, writes=[self.zero.res])
        cT = T("cT_sb", [128, 8, self.NM], F32)
        kb.dma(q, cT[:], self.d["cT"][:, :, :], writes=[cT.res])
        kb.activation(self.silu_c[:], cT[:], AF.Silu, reads=[cT.res], writes=[self.silu_c.res])

    def xr(self, col0, n, out=False):
        rs = self.ores if out else self.xres
        return rs[col0 // 128:(col0 + n + 127) // 128]

    def xview(self, ap, col0, n):
        return ap.rearrange("(c p) t -> p c t", p=128)[:, :, col0:col0 + n]

    def phase_mod(self, i):
        kb = self.kb
        NM = self.NM
        d = self.d
        with contextlib.ExitStack() as st:
            wst = [Buf(kb, st, f"mw{k}", [128, 8, 512], F32) for k in range(2)]
            adab = Buf(kb, st, "adab", [128, 48], F32)
            n1g = Buf(kb, st, "n1g", [128, 8], F32)
            n2g = Buf(kb, st, "n2g", [128, 8], F32)
            kb.dma(kb.q_sp, adab[:], d["ada_bT"][i], writes=[adab.res])
            kb.dma(kb.q_sp, n1g[:], d["n1gT"][i], writes=[n1g.res])
            kb.dma(kb.q_sp, n2g[:], d["n2gT"][i], writes=[n2g.res])
            psm = self.ps[0]
            wv = d["ada_w"][i].rearrange("(kc p) n -> p kc n", p=128)
            for g in range(12):
                w = wst[g % 2]
                kb.dma(kb.q_sp, w[:], wv[:, :, g * 512:(g + 1) * 512], writes=[w.res])
                for jj in range(4):
                    j = g * 4 + jj
                    for kc in range(8):
                        kb.mm(psm[:, j * NM:(j + 1) * NM], w[:, kc, jj * 128:(jj + 1) * 128],
                              self.silu_c[:, kc, :], kc == 0, kc == 7,
                              reads=[w.res, self.silu_c.res], writes=[psm.res])
            kb.tt(kb.dve, self.mod[:], psm[:, 0:48 * NM].rearrange("p (j m) -> p j m", m=NM),
                  adab[:].unsqueeze(2).broadcast_to([128, 48, NM]), ALU.add,
                  reads=[psm.res, adab.res], writes=[self.mod.res])
            for G, base, ng in ((self.G1, 8, n1g), (self.G2, 32, n2g)):
                kb.ts(kb.dve, G[:], self.mod[:, base:base + 8, :], 1.0, None, ALU.add,
                      reads=[self.mod.res], writes=[G.res])
                kb.tt(kb.dve, G[:], G[:], ng[:].unsqueeze(2).broadcast_to([128, 8, NM]), ALU.mult,
                      reads=[G.res, ng.res], writes=[G.res])
            kb.barrier()

    def norm_mod(self, xt, xres, n, mcol, G, sbase, outs, tmp, tmpres, sq, rs, psb):
        kb = self.kb
        kb.activation(sq[:, :, 0:n], xt, AF.Square, reads=[xres], writes=[sq.res])
        for c in range(8):
            kb.mm(psb[:, 0:n], self.ones_bf[:, :], sq[:, c, 0:n], c == 0, c == 7,
                  reads=[sq.res, self.ones_bf.res], writes=[psb.res])
        kb.activation(rs[:, 0:n], psb[:, 0:n], AF.Sqrt, scale=1.0 / D, bias=self.eps[:, 0:1],
                      reads=[psb.res, self.eps.res], writes=[rs.res])
        kb.op(kb.dve, lambda h: h.reciprocal(out=rs[:, 0:n], in_=rs[:, 0:n]), reads=[rs.res], writes=[rs.res])
        kb.tt(kb.dve, tmp, xt, rs[:, 0:n].unsqueeze(1).broadcast_to([128, 8, n]), ALU.mult,
              reads=[xres, rs.res], writes=[tmpres])
        for fn, res in outs:
            for c in range(8):
                if sbase is None:
                    bias = self.zero[:, 0:1]
                    rd = [tmpres, G.res, self.zero.res]
                else:
                    bias = self.mod[:, sbase + c, mcol:mcol + 1]
                    rd = [tmpres, G.res, self.mod.res]
                kb.activation(fn(c), tmp[:, c, :], AF.Identity, scale=G[:, c, mcol:mcol + 1], bias=bias,
                              reads=rd, writes=[res])

    def pack_supers(self, segs, cap=1024):
        supers, cur, tot = [], [], 0
        for s in segs:
            if tot + s.n > cap:
                supers.append(cur)
                cur, tot = [], 0
            cur.append((s, tot))
            tot += s.n
        if cur:
            supers.append(cur)
        return supers

    def phase_ffn(self, i, moe, last, x_src):
        kb = self.kb
        d = self.d
        k = i // 2
        segs = [s for s in self.segs if not (last and s.is_ctx)]
        supers = self.pack_supers(segs)
        NEXP = NE if moe else 1
        if moe:
            W1 = lambda e: d["moe_w1"][k, e]
            W3 = lambda e: d["moe_w3"][k, e]
            W2 = lambda e: d["moe_w2"][k, e]
        else:
            W1 = lambda e: d["ff_w1"][k]
            W3 = lambda e: d["ff_w3"][k]
            W2 = lambda e: d["ff_w2"][k]
        with contextlib.ExitStack() as st:
            T = lambda n, s, dt: Buf(kb, st, n, s, dt)
            h2 = T("h2", [128, 8, 1024], BF16)
            act = T("actb", [128, FC, 1024], BF16)
            xl = T("xl", [128, 8, 512], F32)
            sq = T("sq", [128, 8, 512], BF16)
            rs = T("rs", [128, 512], F32)
            sl = [T(f"sl{j}", [128, 512], F32) for j in range(2)]
            stage = [T(f"stg{j}", [128, 2048], F32) for j in range(2)]
            w13 = [T(f"w13_{j}", [128, 2, 8, 256], BF16) for j in range(2)]
            w2b = [T(f"w2b_{j}", [128, FC, 128], BF16) for j in range(2)]
            yacc = T("yacc", [128, 8, 1024], F32)
            tmpT, tmpres = yacc, yacc.res
            if moe:
                router = T("router", [128, 8, 8], F32)
                kb.dma(kb.q_sp, router[:], d["moe_router"][k].rearrange("(c p) e -> p c e", p=128),
                       writes=[router.res])
                sel = T("sel", [8, 8, 128], F32)
                kb.dma(kb.q_sp, sel[:], d["c_sel"][:, :, :], writes=[sel.res])
                lg = T("lg", [128, 8, 8], F32)
                lg2 = T("lg2", [128, 8, 8], F32)
                mk1 = T("mk1", [128, 8, 8], F32)
                mk2 = T("mk2", [128, 8, 8], F32)
                gate = T("gate", [128, 8, 8], F32)
                sm = T("sm", [128, 6, 8], F32)
                gateT = T("gateT", [8, 1024], F32)
                gbc = [T(f"gbc{j}", [128, 1024], F32) for j in range(1)]
                ytmp = [T(f"ytmp{j}", [128, 512], F32) for j in range(1)]
            stg_i = [0]
            w13_i = [0]
            w2_i = [0]
            cnt = [0]
            for sup in supers:
                Ts = sum(s.n for s, _ in sup)
                nblk = Ts // 128
                for s, off in sup:
                    n = s.n
                    src = x_src if True else None
                    kb.dma(kb.q_sp, xl[:, :, 0:n], self.xview(src, s.col0, n),
                           reads=self.xr(s.col0, n), writes=[xl.res])
                    outs = [(lambda c, off=off, n=n: h2[:, c, off:off + n], h2.res)]
                    if moe:
                        outs.append((lambda c, n=n: xl[:, c, 0:n], xl.res))
                    self.norm_mod(xl[:, :, 0:n], xl.res, n, s.mcol, self.G2, 24, outs,
                                  tmpT[:, :, 0:n], tmpres, sq, rs, self.ps[7])
                    if moe:
                        psl = self.ps[6]
                        for bl in range(n // 128):
                            gb = off // 128 + bl
                            for c in range(8):
                                kb.mm(psl[:, gb * 8:(gb + 1) * 8], xl[:, c, bl * 128:(bl + 1) * 128],
                                      router[:, c, :], c == 0, c == 7,
                                      reads=[xl.res, router.res], writes=[psl.res])
                if moe:
                    dve = kb.dve
                    nb = nblk
                    psl = self.ps[6]
                    kb.copy(dve, lg[:, 0:nb, :], psl[:, 0:nb * 8].rearrange("p (b e) -> p b e", e=8),
                            reads=[psl.res], writes=[lg.res])
                    m1, m2, dd, g1, g2 = (sm[:, j, 0:nb] for j in range(5))
                    bc = lambda a: a.unsqueeze(2).broadcast_to([128, nb, 8])
                    kb.op(dve, lambda h: h.reduce_max(out=m1, in_=lg[:, 0:nb, :], axis=AX.X),
                          reads=[lg.res], writes=[sm.res])
                    kb.tt(dve, mk1[:, 0:nb, :], lg[:, 0:nb, :], bc(m1), ALU.is_equal,
                          reads=[lg.res, sm.res], writes=[mk1.res])
                    kb.stt(dve, lg2[:, 0:nb, :], mk1[:, 0:nb, :], -1e30, lg[:, 0:nb, :], ALU.mult, ALU.add,
                           reads=[mk1.res, lg.res], writes=[lg2.res])
                    kb.op(dve, lambda h: h.reduce_max(out=m2, in_=lg2[:, 0:nb, :], axis=AX.X),
                          reads=[lg2.res], writes=[sm.res])
                    kb.tt(dve, mk2[:, 0:nb, :], lg2[:, 0:nb, :], bc(m2), ALU.is_equal,
                          reads=[lg2.res, sm.res], writes=[mk2.res])
                    kb.tt(dve, dd, m2, m1, ALU.subtract, reads=[sm.res], writes=[sm.res])
                    kb.activation(dd, dd, AF.Exp, reads=[sm.res], writes=[sm.res])
                    kb.ts(dve, g1, dd, 1.0, None, ALU.add, reads=[sm.res], writes=[sm.res])
                    kb.op(dve, lambda h: h.reciprocal(out=g1, in_=g1), reads=[sm.res], writes=[sm.res])
                    kb.tt(dve, g2, dd, g1, ALU.mult, reads=[sm.res], writes=[sm.res])
                    kb.tt(dve, gate[:, 0:nb, :], mk1[:, 0:nb, :], bc(g1), ALU.mult,
                          reads=[mk1.res, sm.res], writes=[gate.res])
                    kb.tt(dve, mk2[:, 0:nb, :], mk2[:, 0:nb, :], bc(g2), ALU.mult,
                          reads=[mk2.res, sm.res], writes=[mk2.res])
                    kb.tt(dve, gate[:, 0:nb, :], gate[:, 0:nb, :], mk2[:, 0:nb, :], ALU.add,
                          reads=[gate.res, mk2.res], writes=[gate.res])
                    for bl in range(nb):
                        pst = self.ps[4 + (bl // 4)]
                        kb.transpose(pst[0:8, (bl % 4) * 128:(bl % 4 + 1) * 128], gate[:, bl, :], self.ident_f[:, :],
                                     reads=[gate.res, self.ident_f.res], writes=[pst.res])
                    for hb in range((nb + 3) // 4):
                        w = min(4, nb - hb * 4) * 128
                        kb.copy(dve, gateT[0:8, hb * 512:hb * 512 + w], self.ps[4 + hb][0:8, 0:w],
                                reads=[self.ps[4 + hb].res], writes=[gateT.res])
                for e in range(NEXP):
                    w1v = W1(e).rearrange("(kc p) f -> p kc f", p=128)
                    w3v = W3(e).rearrange("(kc p) f -> p kc f", p=128)
                    w2v = W2(e).rearrange("(fc p) d -> p fc d", p=128)
                    if moe:
                        gb_t = gbc[0]
                        for s, off in sup:
                            n = s.n
                            pg = self.ps[5]
                            kb.mm(pg[:, 0:n], sel[0:8, e, :], gateT[0:8, off:off + n], True, True,
                                  reads=[sel.res, gateT.res], writes=[pg.res])
                            kb.copy(kb.act, gb_t[:, off:off + n], pg[:, 0:n], reads=[pg.res], writes=[gb_t.res])
                    for fp in range(FC // 2):
                        wb = w13[w13_i[0] % 2]
                        w13_i[0] += 1
                        for which, wv in ((0, w1v), (1, w3v)):
                            sg = stage[stg_i[0] % 2]
                            stg_i[0] += 1
                            sv = sg[:, 0:2048].rearrange("p (kc f) -> p kc f", f=256)
                            kb.dma(kb.q_sp, sv, wv[:, :, fp * 256:(fp + 1) * 256], writes=[sg.res])
                            kb.copy(kb.pool, wb[:, which, :, :], sv, reads=[sg.res], writes=[wb.res])
                        for sub in range(2):
                            fc = fp * 2 + sub
                            for s, off in sup:
                                n = s.n
                                p1 = self.ps[(cnt[0] % 2) * 2]
                                p3 = self.ps[(cnt[0] % 2) * 2 + 1]
                                slt = sl[cnt[0] % 2]
                                cnt[0] += 1
                                for c in range(8):
                                    kb.mm(p1[:, 0:n], wb[:, 0, c, sub * 128:(sub + 1) * 128], h2[:, c, off:off + n],
                                          c == 0, c == 7, reads=[wb.res, h2.res], writes=[p1.res])
                                for c in range(8):
                                    kb.mm(p3[:, 0:n], wb[:, 1, c, sub * 128:(sub + 1) * 128], h2[:, c, off:off + n],
                                          c == 0, c == 7, reads=[wb.res, h2.res], writes=[p3.res])
                                kb.activation(slt[:, 0:n], p1[:, 0:n], AF.Silu, reads=[p1.res], writes=[slt.res])
                                kb.tt(kb.dve, act[:, fc, off:off + n], slt[:, 0:n], p3[:, 0:n], ALU.mult,
                                      reads=[slt.res, p3.res], writes=[act.res])
                    for dc in range(8):
                        wb2 = w2b[w2_i[0] % 2]
                        w2_i[0] += 1
                        for hf in range(2):
                            sg = stage[stg_i[0] % 2]
                            stg_i[0] += 1
                            sv = sg[:, 0:(FC // 2) * 128].rearrange("p (fc d) -> p fc d", d=128)
                            kb.dma(kb.q_sp, sv, w2v[:, hf * (FC // 2):(hf + 1) * (FC // 2), dc * 128:(dc + 1) * 128], writes=[sg.res])
                            kb.copy(kb.pool, wb2[:, hf * (FC // 2):(hf + 1) * (FC // 2), :], sv, reads=[sg.res], writes=[wb2.res])
                        for s, off in sup:
                            n = s.n
                            py = self.ps[cnt[0] % 4]
                            cnt[0] += 1
                            for fc in range(FC):
                                kb.mm(py[:, 0:n], wb2[:, fc, :], act[:, fc, off:off + n], fc == 0, fc == FC - 1,
                                      reads=[wb2.res, act.res], writes=[py.res])
                            if not moe:
                                kb.ts(kb.dve, yacc[:, dc, off:off + n], py[:, 0:n],
                                      self.mod[:, 40 + dc, s.mcol:s.mcol + 1], None, ALU.mult,
                                      reads=[py.res, self.mod.res], writes=[yacc.res])
                            else:
                                if e == 0:
                                    kb.tt(kb.dve, yacc[:, dc, off:off + n], py[:, 0:n], gb_t[:, off:off + n], ALU.mult,
                                          reads=[py.res, gb_t.res], writes=[yacc.res])
                                else:
                                    yt = ytmp[0]
                                    kb.tt(kb.dve, yt[:, 0:n], py[:, 0:n], gb_t[:, off:off + n], ALU.mult,
                                          reads=[py.res, gb_t.res], writes=[yt.res])
                                    kb.tt(kb.pool, yacc[:, dc, off:off + n], yacc[:, dc, off:off + n], yt[:, 0:n],
                                          ALU.add, reads=[yacc.res, yt.res], writes=[yacc.res])
                for s, off in sup:
                    n = s.n
                    kb.dma(kb.q_sp, xl[:, :, 0:n], self.xview(x_src, s.col0, n),
                           reads=self.xr(s.col0, n), writes=[xl.res])
                    if moe:
                        for dc in range(8):
                            kb.stt(kb.dve, xl[:, dc, 0:n], yacc[:, dc, off:off + n],
                                   self.mod[:, 40 + dc, s.mcol:s.mcol + 1], xl[:, dc, 0:n], ALU.mult, ALU.add,
                                   reads=[yacc.res, self.mod.res, xl.res], writes=[xl.res])
                    else:
                        kb.tt(kb.dve, xl[:, :, 0:n], xl[:, :, 0:n], yacc[:, :, off:off + n], ALU.add,
                              reads=[xl.res, yacc.res], writes=[xl.res])
                    kb.dma(kb.q_sp, self.xview(self.xs, s.col0, n), xl[:, :, 0:n],
                           reads=[xl.res], writes=self.xr(s.col0, n))
            kb.barrier()

    def phase_final(self, x_src):
        kb = self.kb
        with contextlib.ExitStack() as st:
            T = lambda n, s, dt: Buf(kb, st, n, s, dt)
            xl = [T(f"fxl{j}", [128, 8, 512], F32) for j in range(2)]
            ot = [T(f"fot{j}", [128, 8, 512], F32) for j in range(2)]
            tmp = T("ftmp", [128, 8, 512], F32)
            sq = T("fsq", [128, 8, 512], BF16)
            rs = T("frs", [128, 512], F32)
            j = 0
            for s in self.segs:
                if s.is_ctx:
                    continue
                n = s.n
                x, o = xl[j % 2], ot[j % 2]
                j += 1
                kb.dma(kb.q_sp, x[:, :, 0:n], self.xview(x_src, s.col0, n), reads=self.xr(s.col0, n), writes=[x.res])
                self.norm_mod(x[:, :, 0:n], x.res, n, 0, self.fg, None,
                              [(lambda c, o=o, n=n: o[:, c, 0:n], o.res)], tmp[:, :, 0:n], tmp.res, sq, rs, self.ps[7])
                kb.dma(kb.q_sp, self.xview(self.out, s.col0, n), o[:, :, 0:n], reads=[o.res],
                       writes=self.xr(s.col0, n, out=True))
            kb.barrier()


def phase_attn(self, i, last):
    kb, d, L, CTX, NB = self.kb, self.d, self.L, self.CTX, self.NB
    j = i // 3
    lam_init = 0.8 - 0.6 * math.exp(-0.3 * i)
    LT = L + CTX
    NKT = LT // 128
    xs = self.xs
    with contextlib.ExitStack() as st:
        T = lambda n, s, dt: Buf(kb, st, n, s, dt)
        hT = T("hT", [128, 8, LT], BF16)
        kraw = T("kraw", [128, max(4 * LT, 6144)], F32)
        vraw = T("vraw", [128, max(NKT * 512, 4096)], F32)
        kT = kraw[:, 0:4 * LT].bitcast(BF16).rearrange("p (h t) -> p h t", h=8)
        tmp = kraw[:, 0:4096].rearrange("p (c t) -> p c t", c=8)
        sq_ap = kraw[:, 4096:6144].bitcast(BF16).rearrange("p (c t) -> p c t", c=8)
        v = vraw[:, 0:NKT * 512].bitcast(BF16).rearrange("p (k e) -> p k e", e=1024)
        xl = vraw[:, 0:4096].rearrange("p (c t) -> p c t", c=8)

        class _V:
            def __init__(s, ap, res):
                s.ap, s.res = ap, res

            def __getitem__(s, k):
                return s.ap[k]
        sq = _V(sq_ap, kraw.res)
        qt = [T(f"qt{k}", [128, 8, 512], BF16) for k in range(2)]
        rcos = T("rcos", [128, L], BF16)
        rsin = T("rsin", [128, L], BF16)
        perm = T("perm", [128, 128], BF16)
        wo = T("wo", [128, 8, 1024], BF16)
        stage = [T(f"astg{k}", [128, 8, 256], F32) for k in range(2)]
        wb = [T(f"awb{k}", [128, 8, 256], BF16) for k in range(2)]
        rs = T("ars", [128, 512], F32)
        ft = [T(f"aft{k}", [128, 512], F32) for k in range(4)]
        bt = [T(f"abt{k}", [128, 512], BF16) for k in range(2)]
        pt = [T(f"apt{k}", [128, 512], BF16) for k in range(4)]
        qst = [T(f"aqst{k}", [128, 512], BF16) for k in range(2)]
        lqk = T("lqk", [64, 4], F32)
        sm = T("asm", [128, 8], F32)
        subg = T("subg", [128, 1], F32)
        q = kb.q_sp
        kb.dma(q, rcos[:], d["c_rope_cos"][:, :], writes=[rcos.res])
        kb.dma(q, rsin[:], d["c_rope_sin"][:, :], writes=[rsin.res])
        kb.dma(q, perm[:], d["c_rope_perm"][:, :], writes=[perm.res])
        kb.dma(q, lqk[:], d["attn_lqk"][j], writes=[lqk.res])
        kb.dma(q, subg[:], d["attn_sublnT"][j], writes=[subg.res])
        kb.tt(kb.dve, sm[0:64, 0:1], lqk[:, 0:1], lqk[:, 1:2], ALU.mult, reads=[lqk.res], writes=[sm.res])
        kb.tt(kb.dve, sm[0:64, 1:2], lqk[:, 2:3], lqk[:, 3:4], ALU.mult, reads=[lqk.res], writes=[sm.res])
        p7 = self.ps[7]
        kb.mm(p7[:, 0:2], self.ones_f[0:64, :], sm[0:64, 0:2], True, True, reads=[self.ones_f.res, sm.res],
              writes=[p7.res])
        kb.activation(sm[:, 2:4], p7[:, 0:2], AF.Exp, reads=[p7.res], writes=[sm.res])
        kb.tt(kb.dve, sm[:, 4:5], sm[:, 3:4], sm[:, 2:3], ALU.subtract, reads=[sm.res], writes=[sm.res])
        kb.ts(kb.dve, sm[:, 4:5], sm[:, 4:5], -lam_init, None, ALU.add, reads=[sm.res], writes=[sm.res])
        neglam = sm[:, 4:5]
        kb.ts(kb.dve, subg[:], subg[:], 1.0 - lam_init, None, ALU.mult, reads=[subg.res], writes=[subg.res])
        wov = d["attn_w_o"][j].rearrange("(kc p) n -> p kc n", p=128)
        sgi = [0]
        for p in range(4):
            sg = stage[sgi[0] % 2]
            sgi[0] += 1
            kb.dma(q, sg[:], wov[:, :, p * 256:(p + 1) * 256], writes=[sg.res])
            kb.copy(kb.pool, wo[:, :, p * 256:(p + 1) * 256], sg[:], reads=[sg.res], writes=[wo.res])
        import os
        STOP = int(os.environ.get('ATT_STOP', '9'))
        wqv = d["attn_w_qkv"][j].rearrange("(kc p) n -> p kc n", p=128)
        qTv = self.qT_d.rearrange("(h p) t -> p h t", p=128)
        cnt = [0]
        for b in range(NB):
            tiles = [s for s in self.segs if s.b == b and not s.is_ctx] + [s for s in self.segs if s.b == b and s.is_ctx]
            offs = {}
            for s in tiles:
                offs[s] = (L if s.is_ctx else s.pos0)
            if STOP < 1:
                break
            for s in tiles:
                n, off = s.n, offs[s]
                kb.dma(q, xl[:, :, 0:n], self.xview(xs, s.col0, n), reads=self.xr(s.col0, n), writes=[vraw.res])
                self.norm_mod(xl[:, :, 0:n], vraw.res, n, s.mcol, self.G1, 0,
                              [(lambda c, off=off, n=n: hT[:, c, off:off + n], hT.res)],
                              tmp[:, :, 0:n], kraw.res, sq, rs, self.ps[7])
            if STOP < 2:
                break
            wi = 0
            A2 = os.environ.get('ATT2', 'qk,v,rope')
            for p in range(12):
                if p < 8 and 'qk' not in A2:
                    continue
                if p >= 8 and 'v' not in A2:
                    continue
                sg = stage[sgi[0] % 2]
                sgi[0] += 1
                w = wb[wi % 2]
                wi += 1
                kb.dma(q, sg[:], wqv[:, :, p * 256:(p + 1) * 256], writes=[sg.res])
                kb.copy(kb.pool, w[:], sg[:], reads=[sg.res], writes=[w.res])
                if p < 8:
                    isq = p < 4
                    for sub in range(2):
                        head = (p % 4) * 2 + sub
                        for s in tiles:
                            n, off = s.n, offs[s]
                            if isq and s.is_ctx and last:
                                continue
                            ps = self.ps[cnt[0] % 4]
                            psr = self.ps[4 + cnt[0] % 2]
                            f1, f2 = ft[(cnt[0] % 2) * 2], ft[(cnt[0] % 2) * 2 + 1]
                            rawb = bt[cnt[0] % 2]
                            qs = qst[cnt[0] % 2]
                            cnt[0] += 1
                            for c in range(8):
                                kb.mm(ps[:, 0:n], w[:, c, sub * 128:(sub + 1) * 128], hT[:, c, off:off + n],
                                      c == 0, c == 7, reads=[w.res, hT.res], writes=[ps.res])
                            if isq:
                                dst, dres = qs[:, 0:n], qs.res
                            else:
                                dst, dres = kT[:, head, off:off + n], kraw.res
                            if not s.is_ctx and 'rope' in A2:
                                pos = s.pos0
                                RV = os.environ.get('ROPEV', 'full')
                                if RV != 'v4':
                                    kb.copy(kb.act, rawb[:, 0:n], ps[:, 0:n], reads=[ps.res], writes=[rawb.res])
                                if RV not in ('v1', 'v2', 'v3', 'v4'):
                                    kb.mm(psr[:, 0:n], perm[:, :], rawb[:, 0:n], True, True,
                                          reads=[perm.res, rawb.res], writes=[psr.res])
                                if RV in ('v2', 'v4'):
                                    kb.copy(kb.dve, f1[:, 0:n], ps[:, 0:n], reads=[ps.res], writes=[f1.res])
                                elif RV == 'v3':
                                    kb.tt(kb.dve, f1[:, 0:n], ps[:, 0:n], rs[:, 0:n], ALU.mult,
                                          reads=[ps.res, rs.res], writes=[f1.res])
                                else:
                                    kb.tt(kb.dve, f1[:, 0:n], ps[:, 0:n], rcos[:, pos:pos + n], ALU.mult,
                                          reads=[ps.res, rcos.res, rawb.res], writes=[f1.res])
                                if RV not in ('v1', 'v2', 'v3', 'v4'):
                                    kb.tt(kb.dve, f2[:, 0:n], psr[:, 0:n], rsin[:, pos:pos + n], ALU.mult,
                                          reads=[psr.res, rsin.res], writes=[f2.res])
                                    kb.tt(kb.dve, dst, f1[:, 0:n], f2[:, 0:n], ALU.add,
                                          reads=[f1.res, f2.res], writes=[dres])
                                else:
                                    kb.copy(kb.dve, dst, f1[:, 0:n], reads=[f1.res], writes=[dres])
                            else:
                                kb.copy(kb.act, dst, ps[:, 0:n], reads=[ps.res], writes=[dres])
                            if isq:
                                kb.dma(kb.q_sp, self.qT_d[head * 128:(head + 1) * 128, off:off + n], qs[:, 0:n],
                                       reads=[qs.res], writes=[self.qres])
                else:
                    vc0 = (p - 8) * 256
                    for tt_ in range(NKT):
                        ps = self.ps[cnt[0] % 4]
                        cnt[0] += 1
                        for c in range(8):
                            kb.mm(ps[:, 0:256], hT[:, c, tt_ * 128:(tt_ + 1) * 128], w[:, c, 0:256],
                                  c == 0, c == 7, reads=[w.res, hT.res], writes=[ps.res])
                        eng = kb.act if tt_ % 2 == 0 else kb.dve
                        kb.copy(eng, v[:, tt_, vc0:vc0 + 256], ps[:, 0:256], reads=[ps.res], writes=[vraw.res])
            if STOP < 4:
                break
            attn = hT
            qi = 0
            for s in tiles:
                n, off = s.n, offs[s]
                if s.is_ctx:
                    if last:
                        continue
                    ktiles = list(range(L // 128, NKT))
                else:
                    ktiles = list(range(NKT))
                qb = qt[qi % 2]
                qi += 1
                kb.dma(q, qb[:, :, 0:n], qTv[:, :, off:off + n], reads=[self.qres], writes=[qb.res])
                for head in range(8):
                    psA = [self.ps[3], self.ps[4]]
                    psU = [self.ps[5], self.ps[6]]
                    for idx, kt in enumerate(ktiles):
                        for comp in range(2):
                            psS = self.ps[cnt[0] % 3]
                            P = pt[cnt[0] % 4]
                            cnt[0] += 1
                            lo, hi = comp * 64, comp * 64 + 64
                            kb.mm(psS[:, 0:n], kT[lo:hi, head, kt * 128:(kt + 1) * 128], qb[lo:hi, head, 0:n],
                                  True, True, reads=[kraw.res, qb.res], writes=[psS.res])
                            kb.activation(P[:, 0:n], psS[:, 0:n], AF.Exp, scale=0.125, reads=[psS.res], writes=[P.res])
                            fst, lst = idx == 0, idx == len(ktiles) - 1
                            kb.mm(psA[comp][:, 0:n], v[:, kt, head * 128:(head + 1) * 128], P[:, 0:n], fst, lst,
                                  reads=[vraw.res, P.res], writes=[psA[comp].res])
                            kb.mm(psU[comp][:, 0:n], self.ones_bf[:, :], P[:, 0:n], fst, lst,
                                  reads=[self.ones_bf.res, P.res], writes=[psU[comp].res])
                    r0, r1, ot = ft[0], ft[1], ft[2]
                    osq = bt[0]
                    dve = kb.dve
                    kb.op(dve, lambda h: h.reciprocal(out=r0[:, 0:n], in_=psU[0][:, 0:n]), reads=[psU[0].res], writes=[r0.res])
                    kb.op(dve, lambda h: h.reciprocal(out=r1[:, 0:n], in_=psU[1][:, 0:n]), reads=[psU[1].res], writes=[r1.res])
                    kb.tt(dve, r0[:, 0:n], psA[0][:, 0:n], r0[:, 0:n], ALU.mult, reads=[psA[0].res, r0.res], writes=[r0.res])
                    kb.tt(dve, r1[:, 0:n], psA[1][:, 0:n], r1[:, 0:n], ALU.mult, reads=[psA[1].res, r1.res], writes=[r1.res])
                    kb.stt(dve, ot[:, 0:n], r1[:, 0:n], neglam, r0[:, 0:n], ALU.mult, ALU.add,
                           reads=[r1.res, r0.res, sm.res], writes=[ot.res])
                    kb.activation(osq[:, 0:n], ot[:, 0:n], AF.Square, reads=[ot.res], writes=[osq.res])
                    kb.mm(p7[:, 0:n], self.ones_bf[:, :], osq[:, 0:n], True, True,
                          reads=[self.ones_bf.res, osq.res], writes=[p7.res])
                    kb.activation(rs[:, 0:n], p7[:, 0:n], AF.Sqrt, scale=1.0 / 128, bias=self.eps[:, 0:1],
                                  reads=[p7.res, self.eps.res], writes=[rs.res])
                    kb.op(dve, lambda h: h.reciprocal(out=rs[:, 0:n], in_=rs[:, 0:n]), reads=[rs.res], writes=[rs.res])
                    kb.tt(dve, ot[:, 0:n], ot[:, 0:n], rs[:, 0:n], ALU.mult, reads=[ot.res, rs.res], writes=[ot.res])
                    kb.activation(attn[:, head, off:off + n], ot[:, 0:n], AF.Identity, scale=subg[:, 0:1],
                                  bias=self.zero[:, 0:1], reads=[ot.res, subg.res, self.zero.res], writes=[hT.res])
            if STOP < 5:
                break
            for s in tiles:
                n, off = s.n, offs[s]
                if s.is_ctx and last:
                    continue
                kb.dma(q, xl[:, :, 0:n], self.xview(xs, s.col0, n), reads=self.xr(s.col0, n), writes=[vraw.res])
                for dc in range(8):
                    ps = self.ps[cnt[0] % 4]
                    cnt[0] += 1
                    for c in range(8):
                        kb.mm(ps[:, 0:n], wo[:, c, dc * 128:(dc + 1) * 128], attn[:, c, off:off + n], c == 0, c == 7,
                              reads=[wo.res, hT.res], writes=[ps.res])
                    kb.stt(kb.dve, xl[:, dc, 0:n], ps[:, 0:n], self.mod[:, 16 + dc, s.mcol:s.mcol + 1], xl[:, dc, 0:n],
                           ALU.mult, ALU.add, reads=[ps.res, self.mod.res, vraw.res], writes=[vraw.res])
                kb.dma(q, self.xview(xs, s.col0, n), xl[:, :, 0:n], reads=[vraw.res], writes=self.xr(s.col0, n))
        kb.barrier()


Builder.phase_attn = phase_attn


def build_all(self):
    kb = self.kb
    for c0 in range(0, self.TOK, 2048):
        n = min(2048, self.TOK - c0)
        kb.dma(kb.q_sp, self.xs[:, c0:c0 + n], self.d["xs_in"][:, c0:c0 + n], writes=self.xr(c0, n))
    import os
    ph = os.environ.get("PHASES", "mod,mix,ffn").split(",")
    for i in range(self.DEPTH):
        last = i == self.DEPTH - 1
        kind = i % 3
        if "mod" in ph:
            self.phase_mod(i)
        if "mix" in ph:
            if kind == 0:
                self.phase_attn(i, last)
            elif kind == 1:
                self.phase_fnet(i, last)
            else:
                self.phase_hyena(i, last)
        if "ffn" in ph:
            self.phase_ffn(i, i % 2 == 1, last, self.xs)
    self.phase_final(self.xs)
    kb.barrier()
    return kb.nc


Builder.build_all = build_all


def prep_inputs(inp, NB, L, CTX, DEPTH, consts, core):
    f32 = np.float32
    b0 = core * NB
    x = np.asarray(inp["x"])[b0:b0 + NB].reshape(NB * L, D)
    cx = np.asarray(inp["ctx"])[b0:b0 + NB].reshape(NB * CTX, D)
    m = {}
    m["xs_in"] = np.ascontiguousarray(np.concatenate([x, cx], 0).T)
    cc = np.concatenate([np.asarray(inp["c"])[b0:b0 + NB], np.asarray(inp["c_ctx"])[None, :]], 0)
    m["cT"] = np.ascontiguousarray(cc.reshape(NB + 1, 8, 128).transpose(2, 1, 0))
    return m


def prep_shared(inp, DEPTH, consts):
    m = {}
    A = lambda k: np.ascontiguousarray(np.asarray(inp[k]), dtype=np.float32)
    m["ada_w"] = A("ada_w")
    m["ada_bT"] = np.ascontiguousarray(A("ada_b").reshape(DEPTH, 48, 128).transpose(0, 2, 1))
    m["n1gT"] = np.ascontiguousarray(A("norm1_g").reshape(DEPTH, 8, 128).transpose(0, 2, 1))
    m["n2gT"] = np.ascontiguousarray(A("norm2_g").reshape(DEPTH, 8, 128).transpose(0, 2, 1))
    m["fgT"] = np.ascontiguousarray(A("final_g").reshape(8, 128).T)
    if "attn_w_qkv" in inp and np.asarray(inp["attn_w_qkv"]).shape[0] > 0:
        m["attn_w_qkv"] = A("attn_w_qkv")
        m["attn_lqk"] = np.ascontiguousarray(np.stack([A("attn_lq1"), A("attn_lk1"), A("attn_lq2"), A("attn_lk2")], -1))
        m["attn_sublnT"] = np.ascontiguousarray(A("attn_subln_g")[:, :, None])
        m["attn_w_o"] = A("attn_w_o")
    if "fnet_w_o" in inp and np.asarray(inp["fnet_w_o"]).shape[0] > 0:
        m["fnet_w_o"] = A("fnet_w_o")
        nb_ = m["fnet_w_o"].shape[0]
        m["fnet_boT"] = np.ascontiguousarray(A("fnet_b_o").reshape(nb_, 8, 128).transpose(0, 2, 1))
    if "hy_w_in" in inp and np.asarray(inp["hy_w_in"]).shape[0] > 0:
        for k in ("hy_w_in", "hy_conv_w", "hy_conv_b", "hy_f_w1", "hy_f_w2", "hy_f_w3", "hy_f_bias", "hy_w_o"):
            m[k] = A(k)
        m["hy_f_b12T"] = np.ascontiguousarray(np.stack([A("hy_f_b1"), A("hy_f_b2")], -1))
    for k in ("ff_w1", "ff_w3", "ff_w2", "moe_router", "moe_w1", "moe_w3", "moe_w2"):
        if k in inp and np.asarray(inp[k]).shape[0] > 0:
            m[k] = A(k)
    for k, v in consts.items():
        m["c_" + k] = v
    return m


def fnet_consts(L, CTX):
    c = {}
    i = np.arange(128)
    ang = 2 * np.pi * np.outer(i, i) / 128.0
    c["fn_cs"] = np.concatenate([np.cos(ang), np.sin(ang)], 1).astype(NPBF)
    for nm, n in (("l", L), ("c", CTX)):
        t = np.arange(n)
        a = 2 * np.pi * (np.outer(t, t) % n) / float(n)
        c["fn_c" + nm] = np.cos(a).astype(NPBF)
        c["fn_ns" + nm] = (-np.sin(a)).astype(NPBF)
    return c


def phase_fnet(self, i, last):
    kb, d, L, CTX, NB = self.kb, self.d, self.L, self.CTX, self.NB
    j = i // 3
    LT = L + CTX
    NKT = LT // 128
    NLT = L // 128
    NCT = CTX // 128
    xs = self.xs
    q = kb.q_sp
    with contextlib.ExitStack() as st:
        T = lambda n, s, dt: Buf(kb, st, n, s, dt)
        hT = T("f_hT", [128, 8, LT], BF16)
        araw = T("f_araw", [128, max(NKT * 8 * 128, 6144 + 4096)], F32)
        A = araw[:, 0:NKT * 8 * 128].bitcast(BF16).rearrange("p (k g e) -> p k g e", g=8, e=256)
        xl = araw[:, 0:4096].rearrange("p (c t) -> p c t", c=8)
        tmp = araw[:, 4096:8192].rearrange("p (c t) -> p c t", c=8)

        class _V:
            def __init__(s, ap, res):
                s.ap, s.res = ap, res

            def __getitem__(s, k):
                return s.ap[k]
        sq = _V(araw[:, 8192:10240].bitcast(BF16).rearrange("p (c t) -> p c t", c=8), araw.res)
        cs = T("f_cs", [128, 256], BF16)
        tabs = [[T(f"f_tab{k}{m}", [128, NLT, 256], BF16) for m in range(2)] for k in range(2)]
        tabc = [T(f"f_tabc{m}", [128, NCT, CTX], BF16) for m in range(2)]
        wo = T("f_wo", [128, 8, 1024], BF16)
        stage = [T(f"f_stg{k}", [128, 8, 256], F32) for k in range(2)]
        rs = T("f_rs", [128, 512], F32)
        ft = [T(f"f_ft{k}", [128, 512], F32) for k in range(2)]
        bo = T("f_bo", [128, 8], F32)
        gb = T("f_gb", [128, 8, self.NM], F32)
        kb.dma(q, cs[:], d["c_fn_cs"][:, :], writes=[cs.res])
        kb.dma(q, bo[:], d["fnet_boT"][j], writes=[bo.res])
        kb.dma(q, tabc[0][:], d["c_fn_cc"].rearrange("(k q) p -> q k p", q=128), writes=[tabc[0].res])
        kb.dma(q, tabc[1][:], d["c_fn_nsc"].rearrange("(k q) p -> q k p", q=128), writes=[tabc[1].res])
        kb.tt(kb.dve, gb[:], self.mod[:, 16:24, :], bo[:].unsqueeze(2).broadcast_to([128, 8, self.NM]), ALU.mult,
              reads=[self.mod.res, bo.res], writes=[gb.res])
        wov = d["fnet_w_o"][j].rearrange("(kc p) n -> p kc n", p=128)
        for p in range(4):
            sg = stage[p % 2]
            kb.dma(q, sg[:], wov[:, :, p * 256:(p + 1) * 256], writes=[sg.res])
            kb.copy(kb.pool, wo[:, :, p * 256:(p + 1) * 256], sg[:], reads=[sg.res], writes=[wo.res])
        tl = [d["c_fn_cl"].rearrange("(k q) p -> q k p", q=128), d["c_fn_nsl"].rearrange("(k q) p -> q k p", q=128)]
        cnt = [0]
        ti = 0
        for b in range(NB):
            tiles = [s for s in self.segs if s.b == b and not s.is_ctx]
            if not last:
                tiles += [s for s in self.segs if s.b == b and s.is_ctx]
            offs = {s: (L if s.is_ctx else s.pos0) for s in tiles}
            for s in tiles:
                n, off = s.n, offs[s]
                kb.dma(q, xl[:, :, 0:n], self.xview(xs, s.col0, n), reads=self.xr(s.col0, n), writes=[araw.res])
                self.norm_mod(xl[:, :, 0:n], araw.res, n, s.mcol, self.G1, 0,
                              [(lambda c, off=off, n=n: hT[:, c, off:off + n], hT.res)],
                              tmp[:, :, 0:n], araw.res, sq, rs, self.ps[7])
            nkt = NKT if not last else NLT
            for tt_ in range(nkt):
                for g in range(0, 8, 2):
                    ps = self.ps[cnt[0] % 4]
                    cnt[0] += 1
                    for gg in range(2):
                        kb.mm(ps[:, gg * 256:(gg + 1) * 256], hT[:, g + gg, tt_ * 128:(tt_ + 1) * 128], cs[:, :],
                              True, True, reads=[hT.res, cs.res], writes=[ps.res])
                    eng = kb.act if (cnt[0] % 2 == 0) else kb.dve
                    kb.copy(eng, A[:, tt_, g:g + 2, :], ps[:, 0:512].rearrange("p (g e) -> p g e", e=256),
                            reads=[ps.res], writes=[araw.res])
            fT = hT
            sc_l = 1.0 / math.sqrt(L * 128.0)
            for pt in range(L // 256):
                tb = tabs[ti % 2]
                ti += 1
                for m in range(2):
                    kb.dma(q, tb[m][:], tl[m][:, :, pt * 256:(pt + 1) * 256], writes=[tb[m].res])
                for g in range(8):
                    ps = self.ps[cnt[0] % 4]
                    cnt[0] += 1
                    k = 0
                    for tt_ in range(NLT):
                        for m in range(2):
                            kb.mm(ps[:, 0:256], A[:, tt_, g, m * 128:(m + 1) * 128], tb[m][:, tt_, :],
                                  k == 0, k == 2 * NLT - 1, reads=[araw.res, tb[m].res], writes=[ps.res])
                            k += 1
                    kb.activation(fT[:, g, pt * 256:(pt + 1) * 256], ps[:, 0:256], AF.Copy, scale=sc_l,
                                  reads=[ps.res], writes=[hT.res])
            if not last:
                sc_c = 1.0 / math.sqrt(CTX * 128.0)
                for g in range(8):
                    ps = self.ps[cnt[0] % 4]
                    cnt[0] += 1
                    k = 0
                    for tt_ in range(NCT):
                        for m in range(2):
                            kb.mm(ps[:, 0:CTX], A[:, NLT + tt_, g, m * 128:(m + 1) * 128], tabc[m][:, tt_, :],
                                  k == 0, k == 2 * NCT - 1, reads=[araw.res, tabc[m].res], writes=[ps.res])
                            k += 1
                    kb.activation(fT[:, g, L:L + CTX], ps[:, 0:CTX], AF.Copy, scale=sc_c,
                                  reads=[ps.res], writes=[hT.res])
            for s in tiles:
                n, off = s.n, offs[s]
                kb.dma(q, xl[:, :, 0:n], self.xview(xs, s.col0, n), reads=self.xr(s.col0, n), writes=[araw.res])
                for dc in range(8):
                    ps = self.ps[cnt[0] % 4]
                    f = ft[cnt[0] % 2]
                    cnt[0] += 1
                    for c in range(8):
                        kb.mm(ps[:, 0:n], wo[:, c, dc * 128:(dc + 1) * 128], fT[:, c, off:off + n], c == 0, c == 7,
                              reads=[wo.res, hT.res], writes=[ps.res])
                    kb.activation(f[:, 0:n], ps[:, 0:n], AF.Identity, scale=self.mod[:, 16 + dc, s.mcol:s.mcol + 1],
                                  bias=gb[:, dc, s.mcol:s.mcol + 1], reads=[ps.res, self.mod.res, gb.res], writes=[f.res])
                    kb.tt(kb.dve, xl[:, dc, 0:n], xl[:, dc, 0:n], f[:, 0:n], ALU.add,
                          reads=[araw.res, f.res], writes=[araw.res])
                kb.dma(q, self.xview(xs, s.col0, n), xl[:, :, 0:n], reads=[araw.res], writes=self.xr(s.col0, n))
        kb.barrier()


Builder.phase_fnet = phase_fnet


def hyena_consts(L, CTX):
    c = {}
    f32 = np.float32
    for nm, Lx in (("l", L), ("c", CTX)):
        N = 2 * Lx
        NT = Lx // 128
        s = np.arange(Lx)
        ang = 2 * np.pi * (np.outer(s, s) % N) / float(N)
        FCm = np.cos(ang)
        FSm = -np.sin(ang)
        FSm[:, 0] = (-1.0) ** s
        ICm = (2.0 / N) * np.cos(ang)
        ICm[0, :] = 1.0 / N
        ISm = -(2.0 / N) * np.sin(ang)
        ISm[0, :] = ((-1.0) ** s) / N

        def blk(M):
            return np.ascontiguousarray(M.reshape(NT, 128, NT, 128).transpose(2, 1, 0, 3)).astype(NPBF)
        c["hy_fc" + nm] = blk(FCm)
        c["hy_fs" + nm] = blk(FSm)
        c["hy_ic" + nm] = blk(ICm)
        c["hy_is" + nm] = blk(ISm)
        t01 = np.linspace(0.0, 1.0, Lx, dtype=f32)[:, None]
        w = (2.0 * math.pi * np.arange(Lx, dtype=f32) / Lx).astype(f32)
        bands = np.linspace(1e-4, 15.0, 16, dtype=f32)
        fw = (w[:, None] * bands[None, :]).astype(f32)
        emb = np.concatenate([t01, np.cos(fw), -np.sin(fw)], -1).astype(f32)
        c["hy_embT" + nm] = np.ascontiguousarray(emb.T)
        mx = math.log(1e-2) / 0.3
        mn = math.log(1e-2) / 1.5
        deltas = np.abs(np.linspace(mn, mx, 1024, dtype=f32))
        c["hy_decay" + nm] = np.exp(-t01 * deltas[None, :]).astype(f32)
    return c


def hy_filters(self, jl, nm, Lx, K_d, kres):
    kb, d = self.kb, self.d
    q = kb.q_sp
    NT = Lx // 128
    TWO_PI = 2.0 * math.pi
    I32 = mybir.dt.int32
    with contextlib.ExitStack() as st:
        T = lambda n, s, dt: Buf(kb, st, n, s, dt)
        embT = T("hf_emb", [33, Lx], F32)
        w1 = T("hf_w1", [33, 64], F32)
        w2 = T("hf_w2", [64, 64], F32)
        w3 = T("hf_w3", [64, 4096], F32)
        bb = T("hf_b", [64, 2], F32)
        h1 = T("hf_h1", [64, Lx], F32)
        h2 = T("hf_h2", [64, Lx], F32)
        r = T("hf_r", [64, 512], F32)
        ri = T("hf_ri", [64, 512], I32)
        rf = T("hf_rf", [64, 512], F32)
        a = T("hf_a", [128, NT, 512], BF16)
        b = T("hf_bb", [128, NT, 512], BF16)
        dec = [T(f"hf_dec{k}", [128, 512], F32) for k in range(2)]
        Ft = [T(f"hf_F{k}", [128, 512], F32) for k in range(2)]
        Bt = [T(f"hf_B{k}", [128, 512], F32) for k in range(2)]
        tabs = [[T(f"hf_tab{k}{m}", [128, NT, 128], BF16) for m in range(2)] for k in range(2)]
        ko = [T(f"hf_ko{k}", [128, 512], F32) for k in range(6)]
        kb.dma(q, embT[:], d["c_hy_embT" + nm][:, :], writes=[embT.res])
        kb.dma(q, w1[:], d["hy_f_w1"][jl], writes=[w1.res])
        kb.dma(q, w2[:], d["hy_f_w2"][jl], writes=[w2.res])
        kb.dma(q, w3[:], d["hy_f_w3"][jl], writes=[w3.res])
        kb.dma(q, bb[:], d["hy_f_b12T"][jl], writes=[bb.res])
        kb.ts(kb.dve, bb[:], bb[:], 1.0 / TWO_PI, None, ALU.mult, reads=[bb.res], writes=[bb.res])
        dve = kb.dve
        for li, (wt, kdim, src, dst) in enumerate(((w1, 33, embT, h1), (w2, 64, h1, h2))):
            for n0 in range(0, Lx, 512):
                n = min(512, Lx - n0)
                ps = self.ps[(n0 // 512) % 2]
                kb.mm(ps[0:64, 0:n], wt[0:kdim, 0:64], src[0:kdim, n0:n0 + n], True, True,
                      reads=[wt.res, src.res], writes=[ps.res])
                kb.activation(r[:, 0:n], ps[0:64, 0:n], AF.Identity, scale=1.0 / TWO_PI, bias=bb[:, li:li + 1],
                              reads=[ps.res, bb.res], writes=[r.res])
                kb.copy(dve, ri[:, 0:n], r[:, 0:n], reads=[r.res], writes=[ri.res])
                kb.copy(dve, rf[:, 0:n], ri[:, 0:n], reads=[ri.res], writes=[rf.res])
                kb.tt(dve, r[:, 0:n], r[:, 0:n], rf[:, 0:n], ALU.subtract, reads=[r.res, rf.res], writes=[r.res])
                kb.ts(dve, rf[:, 0:n], r[:, 0:n], 0.5, None, ALU.is_gt, reads=[r.res], writes=[rf.res])
                kb.tt(dve, r[:, 0:n], r[:, 0:n], rf[:, 0:n], ALU.subtract, reads=[r.res, rf.res], writes=[r.res])
                kb.ts(dve, rf[:, 0:n], r[:, 0:n], -0.5, None, ALU.is_lt, reads=[r.res], writes=[rf.res])
                kb.tt(dve, r[:, 0:n], r[:, 0:n], rf[:, 0:n], ALU.add, reads=[r.res, rf.res], writes=[r.res])
                kb.activation(dst[:, n0:n0 + n], r[:, 0:n], AF.Sin, scale=TWO_PI, reads=[r.res], writes=[dst.res])
        fcv, fsv = d["c_hy_fc" + nm], d["c_hy_fs" + nm]
        dcv = d["c_hy_decay" + nm]
        cnt = 0
        ti = 0
        for o in range(2):
            for dblk in range(2):
                d0 = dblk * 512
                cf = o * 2048 + d0
                cbk = o * 2048 + 1024 + d0
                for tt_ in range(NT):
                    pf, pb = self.ps[2 + (cnt % 2) * 2], self.ps[3 + (cnt % 2) * 2]
                    dc_t, F, B = dec[cnt % 2], Ft[cnt % 2], Bt[cnt % 2]
                    cnt += 1
                    kb.dma(q, dc_t[:], dcv[tt_ * 128:(tt_ + 1) * 128, d0:d0 + 512], writes=[dc_t.res])
                    kb.mm(pf[:, :], h2[0:64, tt_ * 128:(tt_ + 1) * 128], w3[0:64, cf:cf + 512], True, True,
                          reads=[h2.res, w3.res], writes=[pf.res])
                    kb.mm(pb[:, :], h2[0:64, tt_ * 128:(tt_ + 1) * 128], w3[0:64, cbk:cbk + 512], True, True,
                          reads=[h2.res, w3.res], writes=[pb.res])
                    kb.tt(dve, F[:], pf[:, :], dc_t[:], ALU.mult, reads=[pf.res, dc_t.res], writes=[F.res])
                    kb.tt(dve, B[:], pb[:, :], dc_t[:], ALU.mult, reads=[pb.res, dc_t.res], writes=[B.res])
                    if tt_ == 0:
                        kb.memset(dve, B[0:1, :], 0.0, writes=[B.res])
                    kb.tt(kb.pool, a[:, tt_, :], F[:], B[:], ALU.add, reads=[F.res, B.res], writes=[a.res])
                    kb.tt(kb.pool, b[:, tt_, :], F[:], B[:], ALU.subtract, reads=[F.res, B.res], writes=[b.res])
                for kt in range(NT):
                    tb = tabs[ti % 2]
                    ti += 1
                    kb.dma(q, tb[0][:], fcv[kt], writes=[tb[0].res])
                    kb.dma(q, tb[1][:], fsv[kt], writes=[tb[1].res])
                    pA, pB, pC = self.ps[0], self.ps[1], self.ps[6]
                    for st_ in range(NT):
                        kb.mm(pA[:, :], tb[0][:, st_, :], a[:, st_, :], st_ == 0, st_ == NT - 1,
                              reads=[tb[0].res, a.res], writes=[pA.res])
                    for st_ in range(NT):
                        kb.mm(pB[:, :], tb[1][:, st_, :], b[:, st_, :], st_ == 0, st_ == NT - 1,
                              reads=[tb[1].res, b.res], writes=[pB.res])
                    k0, k1, k2 = ko[(kt % 2) * 3], ko[(kt % 2) * 3 + 1], ko[(kt % 2) * 3 + 2]
                    kb.copy(kb.act, k0[:], pA[:, :], reads=[pA.res], writes=[k0.res])
                    kb.copy(dve, k1[:], pB[:, :], reads=[pB.res], writes=[k1.res])
                    rows = slice(kt * 128, (kt + 1) * 128)
                    if kt == 0:
                        for st_ in range(NT):
                            kb.mm(pC[:, :], tb[1][:, st_, :], a[:, st_, :], st_ == 0, st_ == NT - 1,
                                  reads=[tb[1].res, a.res], writes=[pC.res])
                        kb.copy(kb.act, k2[:], k0[:], reads=[k0.res], writes=[k2.res])
                        kb.copy(dve, k2[0:1, :], pC[0:1, :], reads=[pC.res], writes=[k2.res])
                        kb.memset(dve, k1[0:1, :], 0.0, writes=[k1.res])
                        kb.dma(q, K_d[o, 2, rows, d0:d0 + 512], k2[:], reads=[k2.res], writes=[kres])
                    else:
                        kb.dma(q, K_d[o, 2, rows, d0:d0 + 512], k0[:], reads=[k0.res], writes=[kres])
                    kb.dma(q, K_d[o, 0, rows, d0:d0 + 512], k0[:], reads=[k0.res], writes=[kres])
                    kb.dma(q, K_d[o, 1, rows, d0:d0 + 512], k1[:], reads=[k1.res], writes=[kres])
        kb.barrier()


def hy_conv(self, nm, Lx, tok0, K_d, kres, zT, zoff, fb):
    kb, d = self.kb, self.d
    q = kb.q_sp
    NT = Lx // 128
    u_d = self.u_d
    with contextlib.ExitStack() as st:
        T = lambda n, s, dt: Buf(kb, st, n, s, dt)
        x1 = T("hc_x1", [128, NT, 256], F32)
        x2 = T("hc_x2", [128, NT, 256], F32)
        z32 = T("hc_z32", [128, NT, 256], F32)
        zb = T("hc_zb", [128, NT, 256], BF16)
        Yre = T("hc_yre", [128, NT, 256], BF16)
        Yim = T("hc_yim", [128, NT, 256], BF16)
        ftab = [[T(f"hc_ft{k}{m}", [128, NT, 128], BF16) for m in range(2)] for k in range(2)]
        itab = [[T(f"hc_it{k}{m}", [128, NT, 128], BF16) for m in range(2)] for k in range(2)]
        kt_t = [[T(f"hc_k{k}{m}", [128, 256], F32) for m in range(3)] for k in range(2)]
        tm = [T(f"hc_tm{k}", [128, 256], F32) for k in range(5)]
        fcv, fsv = d["c_hy_fc" + nm], d["c_hy_fs" + nm]
        icv, isv = d["c_hy_ic" + nm], d["c_hy_is" + nm]
        uv = u_d[tok0:tok0 + Lx, :].rearrange("(s q) c -> q s c", q=128)
        fi = ii = ki = 0
        cnt = 0
        dve, pool = kb.dve, kb.pool
        for cg in range(4):
            c0 = cg * 256
            kb.dma(q, x1[:], uv[:, :, c0:c0 + 256], reads=[self.ures], writes=[x1.res])
            kb.dma(q, x2[:], uv[:, :, 1024 + c0:1024 + c0 + 256], reads=[self.ures], writes=[x2.res])
            kb.dma(q, z32[:], uv[:, :, 2048 + c0:2048 + c0 + 256], reads=[self.ures], writes=[z32.res])
            kb.copy(pool, zb[:], z32[:], reads=[z32.res], writes=[zb.res])
            for o in range(2):
                gate = x1 if o == 0 else x2
                for kt in range(NT):
                    tb = ftab[fi % 2]
                    fi += 1
                    kk = kt_t[ki % 2]
                    ki += 1
                    kb.dma(q, tb[0][:], fcv[kt], writes=[tb[0].res])
                    kb.dma(q, tb[1][:], fsv[kt], writes=[tb[1].res])
                    for m in range(3):
                        kb.dma(q, kk[m][:], K_d[o, m, kt * 128:(kt + 1) * 128, c0:c0 + 256], reads=[kres],
                               writes=[kk[m].res])
                    pR, pI = self.ps[(cnt % 2) * 2], self.ps[(cnt % 2) * 2 + 1]
                    cnt += 1
                    for st_ in range(NT):
                        kb.mm(pR[:, 0:256], tb[0][:, st_, :], zb[:, st_, :], st_ == 0, st_ == NT - 1,
                              reads=[tb[0].res, zb.res], writes=[pR.res])
                    for st_ in range(NT):
                        kb.mm(pI[:, 0:256], tb[1][:, st_, :], zb[:, st_, :], st_ == 0, st_ == NT - 1,
                              reads=[tb[1].res, zb.res], writes=[pI.res])
                    kb.tt(dve, tm[0][:], pR[:, 0:256], kk[0][:], ALU.mult, reads=[pR.res, kk[0].res], writes=[tm[0].res])
                    kb.tt(dve, tm[1][:], pI[:, 0:256], kk[1][:], ALU.mult, reads=[pI.res, kk[1].res], writes=[tm[1].res])
                    kb.tt(pool, Yre[:, kt, :], tm[0][:], tm[1][:], ALU.subtract, reads=[tm[0].res, tm[1].res],
                          writes=[Yre.res])
                    kb.tt(dve, tm[2][:], pR[:, 0:256], kk[1][:], ALU.mult, reads=[pR.res, kk[1].res], writes=[tm[2].res])
                    kb.tt(dve, tm[3][:], pI[:, 0:256], kk[2][:], ALU.mult, reads=[pI.res, kk[2].res], writes=[tm[3].res])
                    kb.tt(pool, Yim[:, kt, :], tm[2][:], tm[3][:], ALU.add, reads=[tm[2].res, tm[3].res],
                          writes=[Yim.res])
                for tt_ in range(NT):
                    tb = itab[ii % 2]
                    ii += 1
                    kb.dma(q, tb[0][:], icv[tt_], writes=[tb[0].res])
                    kb.dma(q, tb[1][:], isv[tt_], writes=[tb[1].res])
                    pY = self.ps[4 + (cnt % 2)]
                    cnt += 1
                    k = 0
                    for kt in range(NT):
                        for m, Y in ((0, Yre), (1, Yim)):
                            kb.mm(pY[:, 0:256], tb[m][:, kt, :], Y[:, kt, :], k == 0, k == 2 * NT - 1,
                                  reads=[tb[m].res, Y.res], writes=[pY.res])
                            k += 1
                    kb.tt(dve, tm[4][:], z32[:, tt_, :], fb[:, o, c0:c0 + 256], ALU.mult, reads=[z32.res, fb.res],
                          writes=[tm[4].res])
                    kb.tt(dve, tm[4][:], pY[:, 0:256], tm[4][:], ALU.add, reads=[pY.res, tm[4].res], writes=[tm[4].res])
                    kb.tt(pool, z32[:, tt_, :], tm[4][:], gate[:, tt_, :], ALU.mult, reads=[tm[4].res, gate.res],
                          writes=[z32.res])
                    if o == 0:
                        kb.copy(kb.act, zb[:, tt_, :], z32[:, tt_, :], reads=[z32.res], writes=[zb.res])
            for hf in range(2):
                for tg in range(0, NT, 4):
                    w = min(4, NT - tg)
                    pt = self.ps[6]
                    for k in range(w):
                        kb.transpose(pt[:, k * 128:(k + 1) * 128], z32[:, tg + k, hf * 128:(hf + 1) * 128],
                                     self.ident_f[:, :], reads=[z32.res, self.ident_f.res], writes=[pt.res])
                    kb.copy(kb.act, zT[:, cg * 2 + hf, zoff + tg * 128:zoff + (tg + w) * 128], pt[:, 0:w * 128],
                            reads=[pt.res], writes=[zT.res])
        kb.barrier()


def phase_hyena(self, i, last):
    kb, d, L, CTX, NB = self.kb, self.d, self.L, self.CTX, self.NB
    jl = i // 3
    LT = L + CTX
    xs = self.xs
    q = kb.q_sp
    nc = kb.nc
    seqs = [("l", L, 0, 1)]
    if not last:
        seqs.append(("c", CTX, L, L + 3))
    Kd = {}
    for nm, Lx, _, _ in seqs:
        Kd[nm] = (nc.dram_tensor(f"hyK_{nm}_{i}", [2, 3, Lx, 1024], F32).ap(), Res(f"hyK{nm}"))
        hy_filters(self, jl, nm, Lx, Kd[nm][0], Kd[nm][1])
    self.u_d = nc.dram_tensor(f"hy_u_{i}", [LT, 3072], F32).ap()
    self.ures = Res("hy_u")
    with contextlib.ExitStack() as st0:
        T0 = lambda n, s, dt: Buf(kb, st0, n, s, dt)
        hTp = T0("hy_hT", [128, 8, LT + 4], BF16)
        wo = T0("hy_wo", [128, 8, 1024], BF16)
        fb = T0("hy_fb", [128, 2, 1024], F32)
        for o in range(2):
            kb.dma(q, fb[:, o, :], d["hy_f_bias"][jl, o:o + 1, :].partition_broadcast(128), writes=[fb.res])
        for col in (0, L + 1, L + 2, L + CTX + 3):
            kb.memset(kb.dve, hTp[:, :, col:col + 1], 0.0, writes=[hTp.res])
        with contextlib.ExitStack() as st:
            stage = [Buf(kb, st, f"hy_ws{k}", [128, 8, 256], F32) for k in range(2)]
            wov = d["hy_w_o"][jl].rearrange("(kc p) n -> p kc n", p=128)
            for p in range(4):
                sg = stage[p % 2]
                kb.dma(q, sg[:], wov[:, :, p * 256:(p + 1) * 256], writes=[sg.res])
                kb.copy(kb.pool, wo[:, :, p * 256:(p + 1) * 256], sg[:], reads=[sg.res], writes=[wo.res])
            kb.barrier()
        wiv = d["hy_w_in"][jl].rearrange("(kc p) n -> p kc n", p=128)
        cnt = [0]
        for b in range(NB):
            tiles = [s for s in self.segs if s.b == b and not s.is_ctx]
            if not last:
                tiles += [s for s in self.segs if s.b == b and s.is_ctx]
            offs = {s: ((L + 3) if s.is_ctx else 1 + s.pos0) for s in tiles}
            zoffs = {s: (L if s.is_ctx else s.pos0) for s in tiles}
            with contextlib.ExitStack() as st:
                T = lambda n, s, dt: Buf(kb, st, n, s, dt)
                xl = T("hy_xl", [128, 8, 512], F32)
                tmp = T("hy_tmp", [128, 8, 512], F32)
                sq = T("hy_sq", [128, 8, 512], BF16)
                rs = T("hy_rs", [128, 512], F32)
                for s in tiles:
                    n, off = s.n, offs[s]
                    kb.dma(q, xl[:, :, 0:n], self.xview(xs, s.col0, n), reads=self.xr(s.col0, n), writes=[xl.res])
                    self.norm_mod(xl[:, :, 0:n], xl.res, n, s.mcol, self.G1, 0,
                                  [(lambda c, off=off, n=n: hTp[:, c, off:off + n], hTp.res)],
                                  tmp[:, :, 0:n], tmp.res, sq, rs, self.ps[7])
                kb.barrier()
            with contextlib.ExitStack() as st:
                T = lambda n, s, dt: Buf(kb, st, n, s, dt)
                stage = [T(f"hy_st{k}", [128, 8, 256], F32) for k in range(2)]
                wj = [[T(f"hy_wj{k}{m}", [128, 8, 256], BF16) for m in range(3)] for k in range(2)]
                cw = [T(f"hy_cw{k}", [128, 3, 256], F32) for k in range(2)]
                cb = [T(f"hy_cb{k}", [128, 256], F32) for k in range(2)]
                ut = [T(f"hy_ut{k}", [128, 256], F32) for k in range(3)]
                ui = 0
                for p in range(12):
                    c0 = p * 256
                    sg, wjs, cwt, cbt = stage[p % 2], wj[p % 2], cw[p % 2], cb[p % 2]
                    kb.dma(q, sg[:], wiv[:, :, c0:c0 + 256], writes=[sg.res])
                    for m in range(3):
                        kb.dma(q, cwt[:, m, :], d["hy_conv_w"][jl, m:m + 1, c0:c0 + 256].partition_broadcast(128),
                               writes=[cwt.res])
                    kb.dma(q, cbt[:], d["hy_conv_b"][jl:jl + 1, c0:c0 + 256].partition_broadcast(128), writes=[cbt.res])
                    for m in range(3):
                        eng = kb.pool if m == 1 else kb.dve
                        kb.tt(eng, wjs[m][:], sg[:], cwt[:, m, :].unsqueeze(1).broadcast_to([128, 8, 256]), ALU.mult,
                              reads=[sg.res, cwt.res], writes=[wjs[m].res])
                    for nm, Lx, tok0, base in seqs:
                        for tt_ in range(Lx // 128):
                            ps = self.ps[cnt[0] % 4]
                            cnt[0] += 1
                            k = 0
                            for m in range(3):
                                for kc in range(8):
                                    c_lo = base + tt_ * 128 + m - 1
                                    kb.mm(ps[:, 0:256], hTp[:, kc, c_lo:c_lo + 128], wjs[m][:, kc, :], k == 0, k == 23,
                                          reads=[hTp.res, wjs[m].res], writes=[ps.res])
                                    k += 1
                            u = ut[ui % 3]
                            ui += 1
                            kb.tt(kb.dve, u[:], ps[:, 0:256], cbt[:], ALU.add, reads=[ps.res, cbt.res], writes=[u.res])
                            kb.dma(q, self.u_d[tok0 + tt_ * 128:tok0 + (tt_ + 1) * 128, c0:c0 + 256], u[:],
                                   reads=[u.res], writes=[self.ures])
                kb.barrier()
            zT = hTp
            for nm, Lx, tok0, base in seqs:
                hy_conv(self, nm, Lx, tok0, Kd[nm][0], Kd[nm][1], zT, tok0, fb)
            with contextlib.ExitStack() as st:
                xl = Buf(kb, st, "hy_xl4", [128, 8, 512], F32)
                for s in tiles:
                    n, off = s.n, zoffs[s]
                    kb.dma(q, xl[:, :, 0:n], self.xview(xs, s.col0, n), reads=self.xr(s.col0, n), writes=[xl.res])
                    for dc in range(8):
                        ps = self.ps[cnt[0] % 4]
                        cnt[0] += 1
                        for c in range(8):
                            kb.mm(ps[:, 0:n], wo[:, c, dc * 128:(dc + 1) * 128], zT[:, c, off:off + n], c == 0, c == 7,
                                  reads=[wo.res, hTp.res], writes=[ps.res])
                        kb.stt(kb.dve, xl[:, dc, 0:n], ps[:, 0:n], self.mod[:, 16 + dc, s.mcol:s.mcol + 1],
                               xl[:, dc, 0:n], ALU.mult, ALU.add, reads=[ps.res, self.mod.res, xl.res], writes=[xl.res])
                    kb.dma(q, self.xview(xs, s.col0, n), xl[:, :, 0:n], reads=[xl.res], writes=self.xr(s.col0, n))
                for col in (0, L + 1, L + 2, L + CTX + 3):
                    kb.memset(kb.dve, hTp[:, :, col:col + 1], 0.0, writes=[hTp.res])
                kb.barrier()
        kb.barrier()


Builder.phase_hyena = phase_hyena


def kernel(**inputs):
    NB, L, CTX, DEPTH = 4, 2048, 256, 4
    B = Builder(NB, L, CTX, DEPTH)
    nc = B.build_all()
    shared = prep_shared(inputs, DEPTH, B.consts)
    in_maps = []
    for core in range(8):
        m = dict(shared)
        m.update(prep_inputs(inputs, NB, L, CTX, DEPTH, B.consts, core))
        m = {k: v for k, v in m.items() if k in B.d}
        in_maps.append(m)
    res = run_bass_kernel_spmd(nc, in_maps, core_ids=list(range(8)))
    out = np.stack([np.asarray(r["out_T"]).T.reshape(NB, L, 1024) for r in res.results]).reshape(8 * NB, L, 1024)
    return np.ascontiguousarray(out, dtype=np.float32)
```
